# Optimizing a Trainium2 kernel written in Bass

```python
import math
import jax, jax.numpy as jnp
from jax import lax
import numpy as np

D_MODEL = 1024
BATCH = 2
SEQ = 8192
DEPTH = 1

MEM_LEN = 256
NORM_EPS = 1e-6
SSD_HEADS = 8
SSD_HEAD_DIM = 64
SSD_WIDTH = SSD_HEADS * SSD_HEAD_DIM
SSD_GROUPS = 2
SSD_STATE = 128
SSD_CONV = 4
SSD_CHUNK = 128
SSD_CONV_CH = SSD_WIDTH + 2 * SSD_GROUPS * SSD_STATE
MOBA_HEADS = 4
MOBA_HEAD_DIM = 64
MOBA_WIDTH = MOBA_HEADS * MOBA_HEAD_DIM
MOBA_BLOCK = 256
MOBA_TOPK = 3
MOBA_QBLOCK = 128
XATTN_HEADS = 4
XATTN_HEAD_DIM = 64
XATTN_WIDTH = XATTN_HEADS * XATTN_HEAD_DIM
MIX_WIDTH = SSD_WIDTH + MOBA_WIDTH + XATTN_WIDTH
IN_COLS = SSD_WIDTH + SSD_CONV_CH + SSD_HEADS + 3 * MOBA_WIDTH + XATTN_WIDTH
IN_SPLITS = (SSD_WIDTH,
             SSD_WIDTH + SSD_CONV_CH,
             SSD_WIDTH + SSD_CONV_CH + SSD_HEADS,
             SSD_WIDTH + SSD_CONV_CH + SSD_HEADS + MOBA_WIDTH,
             SSD_WIDTH + SSD_CONV_CH + SSD_HEADS + 2 * MOBA_WIDTH,
             SSD_WIDTH + SSD_CONV_CH + SSD_HEADS + 3 * MOBA_WIDTH)
PEER_HEADS = 8
PEER_NKEYS = 128
PEER_EXPERTS = PEER_NKEYS * PEER_NKEYS
PEER_TOPK = 16
PEER_QDIM = 128
PEER_HALF = PEER_QDIM // 2
PEER_CHUNK = 128

kernel_name = 'hybrid_ssd_moba_peer_block'


def rms_norm(x, w):
    xf = x.astype(jnp.float32)
    y = xf * lax.rsqrt(jnp.mean(xf * xf, axis=-1, keepdims=True) + NORM_EPS)
    return (y * w.astype(jnp.float32)).astype(x.dtype)


def alibi_slopes(n):
    return jnp.asarray([2.0 ** (-8.0 * (i + 1) / n) for i in range(n)], dtype=jnp.float32)


def causal_depthwise_conv(u, w, b):
    out = lax.conv_general_dilated(u, w[:, None, :], window_strides=(1,),
                                   padding=[(SSD_CONV - 1, 0)],
                                   dimension_numbers=('NWC', 'WIO', 'NWC'),
                                   feature_group_count=u.shape[-1])
    return out + b


def segsum(a):
    T = a.shape[-1]
    a_rep = jnp.broadcast_to(a[..., :, None], a.shape + (T,))
    a_rep = jnp.where(jnp.tril(jnp.ones((T, T), bool), -1), a_rep, 0.0)
    cs = jnp.cumsum(a_rep, axis=-2)
    return jnp.where(jnp.tril(jnp.ones((T, T), bool), 0), cs, -jnp.inf)


def ssd_chunked(xh, dt, a_coef, bm, cm):
    b, L, h, p = xh.shape
    n = bm.shape[-1]
    nc = L // SSD_CHUNK
    q = SSD_CHUNK
    xdt = (xh * dt[..., None].astype(xh.dtype)).reshape(b, nc, q, h, p)
    bc = bm.reshape(b, nc, q, h, n)
    cc = cm.reshape(b, nc, q, h, n)
    a = (dt * a_coef).reshape(b, nc, q, h).transpose(0, 1, 3, 2)
    a_cs = jnp.cumsum(a, axis=-1)
    decay_in = jnp.exp(segsum(a))
    cb = jnp.einsum('bclhn,bcshn->bchls', cc, bc)
    y_diag = jnp.einsum('bchls,bcshp->bclhp', cb * decay_in, xdt)
    decay_to_end = jnp.exp(a_cs[..., -1:] - a_cs)
    chunk_states = jnp.einsum('bclhn,bchl,bclhp->bchpn', bc, decay_to_end, xdt)
    chunk_decay = jnp.exp(a_cs[..., -1])

    def carry_state(state, inp):
        s_c, d_c = inp
        return state * d_c[..., None, None] + s_c, state

    init = jnp.zeros((b, h, p, n), chunk_states.dtype)
    _, states_in = lax.scan(carry_state, init,
                            (jnp.moveaxis(chunk_states, 1, 0), jnp.moveaxis(chunk_decay, 1, 0)))
    states_in = jnp.moveaxis(states_in, 0, 1)
    y_off = jnp.einsum('bclhn,bchpn,bchl->bclhp', cc, states_in, jnp.exp(a_cs))
    return (y_diag + y_off).reshape(b, L, h, p)


def ssd_mixer(z, xbc, dt_raw, conv_w, conv_b, dt_bias, a_log, d_skip, norm_w):
    b, L, _ = z.shape
    xbc = jax.nn.silu(causal_depthwise_conv(xbc, conv_w, conv_b))
    xs, bm, cm = jnp.split(xbc, [SSD_WIDTH, SSD_WIDTH + SSD_GROUPS * SSD_STATE], axis=-1)
    xh = xs.reshape(b, L, SSD_HEADS, SSD_HEAD_DIM)
    hpg = SSD_HEADS // SSD_GROUPS
    bm = jnp.repeat(bm.reshape(b, L, SSD_GROUPS, SSD_STATE), hpg, axis=2)
    cm = jnp.repeat(cm.reshape(b, L, SSD_GROUPS, SSD_STATE), hpg, axis=2)
    dt = jax.nn.softplus((dt_raw + dt_bias).astype(jnp.float32))
    a_coef = -jnp.exp(a_log.astype(jnp.float32))
    y = ssd_chunked(xh, dt, a_coef, bm, cm) + xh * d_skip[:, None]
    y = y.reshape(b, L, SSD_WIDTH).astype(z.dtype) * jax.nn.silu(z)
    y = rms_norm(y.reshape(b, L, SSD_GROUPS, SSD_WIDTH // SSD_GROUPS),
                 norm_w.reshape(SSD_GROUPS, SSD_WIDTH // SSD_GROUPS))
    return y.reshape(b, L, SSD_WIDTH)


def moba_attention(q, k, v, slopes):
    b, L, h, d = q.shape
    nb = -(-L // MOBA_BLOCK)
    pad = nb * MOBA_BLOCK - L
    scale = 1.0 / math.sqrt(d)
    qt = q.transpose(0, 2, 1, 3)
    kt = jnp.pad(k.transpose(0, 2, 1, 3), ((0, 0), (0, 0), (0, pad), (0, 0)))
    vt = jnp.pad(v.transpose(0, 2, 1, 3), ((0, 0), (0, 0), (0, pad), (0, 0)))
    k_blocks = kt.reshape(b, h, nb, MOBA_BLOCK, d)
    v_blocks = vt.reshape(b, h, nb, MOBA_BLOCK, d)
    k_mean = jnp.mean(k_blocks.astype(jnp.float32), axis=3)
    gate = jnp.einsum('bhtd,bhnd->bhtn', qt.astype(jnp.float32), k_mean)
    q_blk = jnp.arange(L) // MOBA_BLOCK
    past = jnp.arange(nb)[None, :] < q_blk[:, None]
    gate = jnp.where(past, gate, -jnp.inf)
    n_sel = min(MOBA_TOPK, nb)
    _, sel = lax.top_k(gate, n_sel)
    ncq = L // MOBA_QBLOCK
    q_chunks = jnp.moveaxis(qt.reshape(b, h, ncq, MOBA_QBLOCK, d), 2, 0)
    sel_chunks = jnp.moveaxis(sel.reshape(b, h, ncq, MOBA_QBLOCK, n_sel), 2, 0)
    bi = jnp.arange(b)[:, None, None, None]
    hi = jnp.arange(h)[None, :, None, None]

    def attend(args):
        qc, selc, ci = args
        t = ci * MOBA_QBLOCK + jnp.arange(MOBA_QBLOCK)
        own = (ci * MOBA_QBLOCK) // MOBA_BLOCK
        ks = k_blocks[bi, hi, selc]
        vs = v_blocks[bi, hi, selc]
        s_pos = selc[..., None] * MOBA_BLOCK + jnp.arange(MOBA_BLOCK)
        s_sel = jnp.einsum('bhqd,bhqjkd->bhqjk', qc, ks) * scale
        dist_sel = jnp.abs(t[:, None, None] - s_pos).astype(jnp.float32)
        s_sel = s_sel - slopes[:, None, None, None] * dist_sel
        valid = jnp.arange(n_sel) < own
        s_sel = jnp.where(valid[:, None], s_sel, -jnp.inf)
        k_own = lax.dynamic_index_in_dim(k_blocks, own, axis=2, keepdims=False)
        v_own = lax.dynamic_index_in_dim(v_blocks, own, axis=2, keepdims=False)
        own_pos = own * MOBA_BLOCK + jnp.arange(MOBA_BLOCK)
        s_own = jnp.einsum('bhqd,bhkd->bhqk', qc, k_own) * scale
        dist_own = jnp.abs(t[:, None] - own_pos[None, :]).astype(jnp.float32)
        s_own = s_own - slopes[:, None, None] * dist_own
        s_own = jnp.where(own_pos[None, :] <= t[:, None], s_own, -jnp.inf)
        scores = jnp.concatenate(
            [s_sel.reshape(b, h, MOBA_QBLOCK, n_sel * MOBA_BLOCK), s_own], axis=-1).astype(jnp.float32)
        p = jax.nn.softmax(scores, axis=-1).astype(v.dtype)
        p_sel = p[..., :n_sel * MOBA_BLOCK].reshape(b, h, MOBA_QBLOCK, n_sel, MOBA_BLOCK)
        p_own = p[..., n_sel * MOBA_BLOCK:]
        return (jnp.einsum('bhqjk,bhqjkd->bhqd', p_sel, vs)
                + jnp.einsum('bhqk,bhkd->bhqd', p_own, v_own))

    out = lax.map(attend, (q_chunks, sel_chunks, jnp.arange(ncq)))
    out = jnp.moveaxis(out, 0, 2).reshape(b, h, L, d)
    return out.transpose(0, 2, 1, 3)


def memory_cross_attention(q, mem_k, mem_v):
    scale = 1.0 / math.sqrt(q.shape[-1])
    s = jnp.einsum('blhd,bmhd->bhlm', q, mem_k).astype(jnp.float32) * scale
    p = jax.nn.softmax(s, axis=-1).astype(mem_v.dtype)
    return jnp.einsum('bhlm,bmhd->blhd', p, mem_v)


def peer_ffn(xn, w_query, sub_keys_1, sub_keys_2, expert_down, expert_up):
    b, L, _ = xn.shape
    q = jnp.einsum('bld,dk->blk', xn, w_query).reshape(b, L, PEER_HEADS, 2, PEER_HALF)
    s1 = jnp.einsum('blhd,nd->blhn', q[..., 0, :], sub_keys_1)
    s2 = jnp.einsum('blhd,nd->blhn', q[..., 1, :], sub_keys_2)
    v1, i1 = lax.top_k(s1, PEER_TOPK)
    v2, i2 = lax.top_k(s2, PEER_TOPK)
    cand_s = (v1[..., :, None] + v2[..., None, :]).reshape(b, L, PEER_HEADS, PEER_TOPK * PEER_TOPK)
    cand_i = (i1[..., :, None] * PEER_NKEYS + i2[..., None, :]).reshape(b, L, PEER_HEADS, PEER_TOPK * PEER_TOPK)
    top_s, top_pos = lax.top_k(cand_s, PEER_TOPK)
    e_idx = jnp.take_along_axis(cand_i, top_pos, axis=-1)
    gates = jax.nn.softmax(top_s.astype(jnp.float32), axis=-1).astype(xn.dtype)
    nchunk = L // PEER_CHUNK

    def to_chunks(t):
        return jnp.moveaxis(t.reshape((b, nchunk, PEER_CHUNK) + t.shape[2:]), 1, 0)

    def expert_block(args):
        xc, ic, gc = args
        u = expert_down[ic]
        act = jax.nn.gelu(jnp.einsum('bthkd,btd->bthk', u, xc), approximate=False)
        w_up = expert_up[ic]
        return jnp.einsum('bthk,bthkd->btd', gc * act, w_up)

    out = lax.map(expert_block, (to_chunks(xn), to_chunks(e_idx), to_chunks(gates)))
    return jnp.moveaxis(out, 0, 1).reshape(b, L, D_MODEL)


def setup_inputs(seed: int = 0) -> dict:
    key = jax.random.key(seed)
    ks = jax.random.split(key, 24)
    f32 = jnp.float32

    def nrm(k, shape, scale):
        return jax.random.normal(k, shape, f32) * scale

    def gain(k, shape):
        return 1.0 + 0.05 * jax.random.normal(k, shape, f32)

    dt0 = jnp.exp(jax.random.uniform(ks[5], (DEPTH, SSD_HEADS), f32, math.log(1e-3), math.log(1e-1)))
    return {
        'x': nrm(ks[0], (BATCH, SEQ, D_MODEL), 1.0),
        'mem': nrm(ks[1], (BATCH, MEM_LEN, D_MODEL), 1.0),
        'mix_norm_w': gain(ks[2], (DEPTH, D_MODEL)),
        'w_in': nrm(ks[3], (DEPTH, D_MODEL, IN_COLS), D_MODEL ** -0.5),
        'ssd_conv_w': nrm(ks[4], (DEPTH, SSD_CONV, SSD_CONV_CH), 0.5),
        'ssd_conv_b': nrm(ks[6], (DEPTH, SSD_CONV_CH), 0.02),
        'ssd_dt_bias': dt0 + jnp.log(-jnp.expm1(-dt0)),
        'ssd_a_log': jnp.log(jax.random.uniform(ks[7], (DEPTH, SSD_HEADS), f32, 1.0, 16.0)),
        'ssd_d': gain(ks[8], (DEPTH, SSD_HEADS)),
        'ssd_norm_w': gain(ks[9], (DEPTH, SSD_WIDTH)),
        'moba_q_norm_w': gain(ks[10], (DEPTH, MOBA_HEAD_DIM)),
        'moba_k_norm_w': gain(ks[11], (DEPTH, MOBA_HEAD_DIM)),
        'mem_norm_w': gain(ks[12], (DEPTH, D_MODEL)),
        'w_mem_kv': nrm(ks[13], (DEPTH, D_MODEL, 2 * XATTN_WIDTH), D_MODEL ** -0.5),
        'xattn_q_norm_w': gain(ks[14], (DEPTH, XATTN_HEAD_DIM)),
        'xattn_k_norm_w': gain(ks[15], (DEPTH, XATTN_HEAD_DIM)),
        'w_out': nrm(ks[16], (DEPTH, MIX_WIDTH, D_MODEL), MIX_WIDTH ** -0.5),
        'ffn_norm_w': gain(ks[17], (DEPTH, D_MODEL)),
        'peer_w_query': nrm(ks[18], (DEPTH, D_MODEL, PEER_HEADS * PEER_QDIM), D_MODEL ** -0.5),
        'peer_sub_keys_1': nrm(ks[19], (DEPTH, PEER_NKEYS, PEER_HALF), PEER_HALF ** -0.5),
        'peer_sub_keys_2': nrm(ks[20], (DEPTH, PEER_NKEYS, PEER_HALF), PEER_HALF ** -0.5),
        'peer_expert_down': nrm(ks[21], (DEPTH, PEER_EXPERTS, D_MODEL), D_MODEL ** -0.5),
        'peer_expert_up': nrm(ks[22], (DEPTH, PEER_EXPERTS, D_MODEL), PEER_HEADS ** -0.5),
    }


def reference(x, mem, mix_norm_w, w_in, ssd_conv_w, ssd_conv_b, ssd_dt_bias, ssd_a_log, ssd_d,
              ssd_norm_w, moba_q_norm_w, moba_k_norm_w, mem_norm_w, w_mem_kv, xattn_q_norm_w,
              xattn_k_norm_w, w_out, ffn_norm_w, peer_w_query, peer_sub_keys_1, peer_sub_keys_2,
              peer_expert_down, peer_expert_up):
    b, L, _ = x.shape
    slopes = alibi_slopes(MOBA_HEADS)
    h = x
    for l in range(DEPTH):
        xn = rms_norm(h, mix_norm_w[l])
        proj = jnp.einsum('bld,dk->blk', xn, w_in[l])
        z, xbc, dt_raw, mq, mk, mv, xq = jnp.split(proj, list(IN_SPLITS), axis=-1)
        y_ssd = ssd_mixer(z, xbc, dt_raw, ssd_conv_w[l], ssd_conv_b[l], ssd_dt_bias[l],
                          ssd_a_log[l], ssd_d[l], ssd_norm_w[l])
        mq = rms_norm(mq.reshape(b, L, MOBA_HEADS, MOBA_HEAD_DIM), moba_q_norm_w[l])
        mk = rms_norm(mk.reshape(b, L, MOBA_HEADS, MOBA_HEAD_DIM), moba_k_norm_w[l])
        mv = mv.reshape(b, L, MOBA_HEADS, MOBA_HEAD_DIM)
        y_moba = moba_attention(mq, mk, mv, slopes).reshape(b, L, MOBA_WIDTH)
        mem_n = rms_norm(mem, mem_norm_w[l])
        mem_kv = jnp.einsum('bmd,dk->bmk', mem_n, w_mem_kv[l])
        mem_k, mem_v = jnp.split(mem_kv, 2, axis=-1)
        M = mem.shape[1]
        mem_k = rms_norm(mem_k.reshape(b, M, XATTN_HEADS, XATTN_HEAD_DIM), xattn_k_norm_w[l])
        mem_v = mem_v.reshape(b, M, XATTN_HEADS, XATTN_HEAD_DIM)
        xq = rms_norm(xq.reshape(b, L, XATTN_HEADS, XATTN_HEAD_DIM), xattn_q_norm_w[l])
        y_mem = memory_cross_attention(xq, mem_k, mem_v).reshape(b, L, XATTN_WIDTH)
        mixed = jnp.concatenate([y_ssd, y_moba, y_mem], axis=-1)
        h = h + jnp.einsum('blk,kd->bld', mixed, w_out[l])
        hn = rms_norm(h, ffn_norm_w[l])
        h = h + peer_ffn(hn, peer_w_query[l], peer_sub_keys_1[l], peer_sub_keys_2[l],
                         peer_expert_down[l], peer_expert_up[l])
    return h
```

```python
import os
import numpy as np
from contextlib import ExitStack
import concourse.bass as bass
import concourse.mybir as mybir
from concourse.bass_utils import run_bass_kernel_spmd

F32 = mybir.dt.float32
BF16 = mybir.dt.bfloat16
U32 = mybir.dt.uint32
I32 = mybir.dt.int32
ALU = mybir.AluOpType
AF = mybir.ActivationFunctionType
AX = mybir.AxisListType

L = 8192
D = 1024
NT = L // 128
NT2 = 16
EPS = 1e-6
NEG = 30000.0
NBUF = 6


class MK:
    def __init__(self, nc, es):
        self.nc = nc
        self.es = es
        self.eng = {'pe': nc.tensor, 'act': nc.scalar, 'dve': nc.vector, 'pool': nc.gpsimd, 'sp': nc.sync}
        self.sem = {k: es.enter_context(nc.semaphore('s_' + k)) for k in self.eng}
        self.cnt = {k: 0 for k in self.eng}
        self.waited = {k: {} for k in self.eng}
        self.res = {}
        self.chan = {}
        self.stopped = False
        self.serial = True
        al = {'trp': 'B_trp', 'fm': 'B_fm', 'tm': 'B_tm'}
        for k in ('p_gt', 'p_dt0', 'p_dt1', 'p_acs'):
            al[k] = 'B_sa'
        for k in ('p_y0', 'p_y1', 'p_yo0', 'p_yo1', 'p_sn0', 'p_sn1'):
            al[k] = 'B_sb'
        for k in ('p_xt', 'p_bt', 'p_qT', 'p_kT'):
            al[k] = 'B_sc'
        for k in ('p_sc', 'p_xs'):
            al[k] = 'B_at'
        for k in ('p_om0', 'p_ox', 'p_gate', 'p_km'):
            al[k] = 'B_ao'
        al['p_om1'] = 'B_tm'
        al['p_xqT'] = 'B_sb'
        al['p_nmT'] = 'B_fm'
        self.alias = al

    def _al(self, keys):
        return [self.alias.get(k, k) if isinstance(k, str) else k for k in keys]

    def _st(self, key):
        if key not in self.res:
            self.res[key] = {'w': None, 'r': []}
        return self.res[key]

    def _wait(self, e, ev):
        if ev is None or self.stopped:
            return
        sid, sem, val, src = ev
        if self.waited[e].get(sid, 0) >= val:
            return
        self.waited[e][sid] = val
        self.eng[e].wait_ge(sem, val)

    def _deps(self, e, reads, writes):
        for k in reads:
            ev = self._st(k)['w']
            if ev is not None and not (e == 'pe' and ev[3] == 'pe'):
                self._wait(e, ev)
        for k in writes:
            st = self._st(k)
            ev = st['w']
            if ev is not None and ev[3] != e:
                self._wait(e, ev)
            for ev in st['r']:
                if ev[3] != e:
                    self._wait(e, ev)

    def _commit(self, ev, reads, writes):
        for k in reads:
            st = self._st(k)
            st['r'] = [x for x in st['r'] if not (x[0] == ev[0])] + [ev]
        for k in writes:
            st = self._st(k)
            st['w'] = ev
            st['r'] = []

    def op(self, e, fn, reads=(), writes=()):
        if self.stopped:
            return None
        reads = self._al(reads)
        writes = self._al(writes)
        self._deps(e, reads, writes)
        if self.serial:
            for e2 in ('pe', 'act', 'dve', 'pool'):
                if e2 != e and self.cnt[e2] > 0:
                    self._wait(e, ('e_' + e2, self.sem[e2], self.cnt[e2], e2))
        ins = fn(self.eng[e])
        self.cnt[e] += 1
        ins.then_inc(self.sem[e], 1)
        ev = ('e_' + e, self.sem[e], self.cnt[e], e)
        self._commit(ev, reads, writes)
        return ins

    def dma(self, q, fn, chan, reads=(), writes=()):
        if self.stopped:
            return None
        reads = self._al(reads)
        writes = self._al(writes)
        if chan not in self.chan:
            self.chan[chan] = [self.es.enter_context(self.nc.semaphore('c%d' % len(self.chan))), 0]
        c = self.chan[chan]
        ck = ('chan', chan)
        self._deps(q, reads, list(writes) + [ck])
        ins = fn(self.eng[q])
        c[1] += 16
        ins.then_inc(c[0], 16)
        ev = ('c_' + str(chan), c[0], c[1], 'dma')
        self._commit(ev, reads, list(writes) + [ck])
        return ins

    def finish(self, e, keys):
        for k in keys:
            self._wait(e, self._st(k)['w'])


class _Stop(Exception):
    pass


def build(debug=False, nt1=NT, nt2=NT2, noexp=False, stop=0):
    stf = {'mk': None}

    def ckl(i, n):
        if i == nt1 - 1:
            ck(n)

    def ck(n):
        if stop == n:
            stf['mk'].stopped = True

    nc = bass.Bass("TRN2", target_bir_lowering=False)

    def din(name, shape, dt=F32):
        return nc.dram_tensor(name, list(shape), dt, kind="ExternalInput").ap()

    x_full = din("x_full", [L, D])
    x_res = din("x_res", [NT2 * 128, D])
    w_in = din("w_in", [D, 770])
    mixw = din("mixw", [1, D])
    convw = din("convw", [128, 12])
    convb = din("convb", [128, 3])
    ssdv = din("ssdv", [1, 6])
    qkw = din("qkw", [1, 192])
    mem = din("mem", [256, D])
    memw = din("memw", [1, D])
    wkv = din("wkv", [D, 128])
    xkw = din("xkw", [1, 64])
    alibi = din("alibi", [128, 65])
    w_out = din("w_out", [D, D])
    ssdnw = din("ssdnw", [1, 512])
    ridx = din("ridx", [128, NT2 * 4], I32)
    ffnw = din("ffnw", [1, D])
    wq = din("wq", [D, D])
    kk = din("kk", [128, 256])
    e_dn = din("e_dn", [16384, D])
    e_up = din("e_up", [16384, D])
    out = nc.dram_tensor("out", [NT2 * 128, D], F32, kind="ExternalOutput").ap()
    ag_in = nc.dram_tensor("ag_in", [L, 256], F32)
    ag_out = nc.dram_tensor("ag_out", [4 * L, 256], F32)
    if debug:
        dbg_mixed = nc.dram_tensor("dbg_mixed", [L, 256], F32, kind="ExternalOutput").ap()
        dbg_h = nc.dram_tensor("dbg_h", [NT2 * 128, D], F32, kind="ExternalOutput").ap()
        dbg_q = nc.dram_tensor("dbg_q", [128, 1024], F32, kind="ExternalOutput").ap()

    with ExitStack() as es0:
        mk = MK(nc, es0)
        stf['mk'] = mk

        def V(fn, r=(), w=()):
            return mk.op('dve', fn, r, w)

        def A(fn, r=(), w=()):
            return mk.op('act', fn, r, w)

        def P(fn, r=(), w=()):
            return mk.op('pe', fn, r, w)

        def G(fn, r=(), w=()):
            return mk.op('pool', fn, r, w)

        def DMA(out_, in_, chan, r=(), w=(), q='sp'):
            return mk.dma(q, lambda e: e.dma_start(out=out_, in_=in_), chan, r, w)

        def sb0(name, shape, dt=F32):
            return es0.enter_context(nc.sbuf_tensor(name, list(shape), dt))

        idf = sb0("idf", [128, 128])
        idb = sb0("idb", [128, 128], BF16)
        iof = sb0("iof", [128, 128])
        iop = sb0("iop", [128, 1])
        G(lambda e: e.iota(iof[:], pattern=[[1, 128]], base=0, channel_multiplier=0,
                           allow_small_or_imprecise_dtypes=True), w=['iof'])
        G(lambda e: e.iota(iop[:], pattern=[[1, 1]], base=0, channel_multiplier=1,
                           allow_small_or_imprecise_dtypes=True), w=['iop'])
        V(lambda e: e.tensor_scalar(out=idf[:], in0=iof[:], scalar1=iop[:, 0:1], scalar2=None, op0=ALU.is_equal),
          r=['iof', 'iop'], w=['idf'])
        V(lambda e: e.tensor_copy(out=idb[:], in_=idf[:]), r=['idf'], w=['idb'])

        def rstd_inplace(ap, n, key):
            A(lambda e: e.activation(out=ap, in_=ap, func=AF.Sqrt, bias=EPS, scale=1.0 / n), r=[key], w=[key])
            V(lambda e: e.reciprocal(out=ap, in_=ap), r=[key], w=[key])

        try:
            ck(1)
            with ExitStack() as es1:
                def sb(name, shape, dt=F32):
                    return es1.enter_context(nc.sbuf_tensor(name, list(shape), dt))

                def psb(name):
                    return es1.enter_context(nc.psum_tensor(name, [128, 512], F32))

                pb_trp = psb("pb_trp")
                pb_fm = psb("pb_fm")
                pb_tm = psb("pb_tm")
                pb_sa = psb("pb_sa")
                pb_sb = psb("pb_sb")
                pb_sc = psb("pb_sc")
                pb_at = psb("pb_at")
                pb_ao = psb("pb_ao")
                trp = pb_trp[:, 0:512].bitcast(BF16)
                fm = [pb_fm[:, i * 128:(i + 1) * 128] for i in range(3)]
                tm = pb_tm[:, 0:386]
                p_gt = pb_sa[:, 0:128]
                p_dt = [pb_sa[:, 128:256], pb_sa[:, 256:384]]
                p_acs = pb_sa[:, 384:388]
                p_y = [pb_sb[:, 0:64], pb_sb[:, 64:128]]
                p_yo = [pb_sb[:, 128:192], pb_sb[:, 192:256]]
                p_sn = [pb_sb[:, 256:320], pb_sb[:, 320:384]]
                p_xt = pb_sc[:, 0:128]
                p_bt = pb_sc[:, 128:192].bitcast(BF16)
                p_qT = pb_sc[0:64, 192:320]
                p_kT = pb_sc[0:64, 320:448]
                p_sc = pb_at[:, 0:256]
                p_xs = pb_at[:, 256:512]
                p_om = [pb_ao[:, 0:65], pb_tm[:, 400:465]]
                p_ox = pb_ao[:, 256:321]
                p_gate = pb_ao[:, 384:416]
                p_km = pb_ao[0:64, 448:449]
                p_xqT = pb_sb[0:64, 384:512]
                p_nmT = pb_fm[0:32, 384:512]

                Um = sb("Um", [128, 128])
                Ls = sb("Ls", [128, 128])
                ones = sb("ones", [128, 128])
                onesc = sb("onesc", [128, 1])
                V(lambda e: e.tensor_scalar(out=Um[:], in0=iof[:], scalar1=iop[:, 0:1], scalar2=None, op0=ALU.is_ge),
                  r=['iof', 'iop'], w=['Um'])
                V(lambda e: e.tensor_scalar(out=Ls[:], in0=iof[:], scalar1=iop[:, 0:1], scalar2=None, op0=ALU.is_lt),
                  r=['iof', 'iop'], w=['Ls'])
                V(lambda e: e.memset(ones[:], 1.0), w=['ones'])
                V(lambda e: e.memset(onesc[:], 1.0 / 256.0), w=['onesc'])
                cm0 = sb("cm0", [128, 256], BF16)
                cm1 = sb("cm1", [128, 256], BF16)
                V(lambda e: e.tensor_scalar(out=cm0[:, 0:128], in0=Um[:], scalar1=-1.0, scalar2=None, op0=ALU.add),
                  r=['Um'], w=['cm0'])
                V(lambda e: e.tensor_scalar(out=cm0[:, 128:256], in0=Um[:], scalar1=0.0, scalar2=None, op0=ALU.mult), r=['Um'], w=['cm0'])
                V(lambda e: e.tensor_scalar(out=cm1[:, 0:128], in0=Um[:], scalar1=0.0, scalar2=-1.0, op0=ALU.mult, op1=ALU.add), r=['Um'], w=['cm1'])
                V(lambda e: e.tensor_scalar(out=cm1[:, 128:256], in0=Um[:], scalar1=-1.0, scalar2=None, op0=ALU.add),
                  r=['Um'], w=['cm1'])
                i30 = sb("i30", [128, 128], BF16)
                V(lambda e: e.tensor_scalar(out=i30[:], in0=idf[:], scalar1=NEG, scalar2=None, op0=ALU.mult),
                  r=['idf'], w=['i30'])
                sel30 = sb("sel30", [32, 32, 128], BF16)
                self_f = sb("self_f", [32, 32, 128])
                G(lambda e: e.iota(self_f[:], pattern=[[1, 32], [0, 128]], base=0, channel_multiplier=-1,
                                   allow_small_or_imprecise_dtypes=True), w=['self_f'])
                V(lambda e: e.tensor_scalar(out=sel30[:], in0=self_f[:], scalar1=0.0, scalar2=NEG, op0=ALU.is_equal,
                                            op1=ALU.mult), r=['self_f'], w=['sel30'])
                alib = sb("alib", [128, 65])
                DMA(alib[:], alibi, 'alib', w=['alib'])

                wb = sb("wb", [128, D])
                DMA(wb[:], mixw.partition_broadcast(128), 'wb', w=['wb'])
                cw = sb("cw", [128, 12])
                cb = sb("cb", [128, 3])
                DMA(cw[:], convw, 'cw', w=['cw'])
                DMA(cb[:], convb, 'cb', w=['cb'])
                sv = sb("sv", [128, 6])
                DMA(sv[:], ssdv.partition_broadcast(128), 'sv', w=['sv'])
                acoef = sb("acoef", [128, 2])
                A(lambda e: e.activation(out=acoef[:], in_=sv[:, 2:4], func=AF.Exp), r=['sv'], w=['acoef'])
                V(lambda e: e.tensor_scalar(out=acoef[:], in0=acoef[:], scalar1=-1.0, scalar2=None, op0=ALU.mult),
                  r=['acoef'], w=['acoef'])
                qkwb = sb("qkwb", [128, 192])
                DMA(qkwb[:], qkw.partition_broadcast(128), 'qkwb', w=['qkwb'])
                xkwb = sb("xkwb", [128, 64])
                DMA(xkwb[:], xkw.partition_broadcast(128), 'xkwb', w=['xkwb'])
                memwb = sb("memwb", [128, D])
                DMA(memwb[:], memw.partition_broadcast(128), 'memwb', w=['memwb'])

                ck(2)
                w_in_b = sb("w_in_b", [128, 8, 770], BF16)
                wkv_b = sb("wkv_b", [128, 8, 128], BF16)
                wst = [sb("wst%d" % i, [128, 770]) for i in range(2)]
                for k in range(8):
                    s = wst[k % 2]
                    sk = 'wst%d' % (k % 2)
                    DMA(s[:], w_in[k * 128:(k + 1) * 128, :], sk, w=[sk])
                    A(lambda e: e.copy(out=w_in_b[:, k, :], in_=s[:]), r=[sk], w=['w_in_b'])
                for k in range(8):
                    s = wst[k % 2]
                    sk = 'wst%d' % (k % 2)
                    DMA(s[:, 0:128], wkv[k * 128:(k + 1) * 128, :], sk, w=[sk])
                    A(lambda e: e.copy(out=wkv_b[:, k, :], in_=s[:, 0:128]), r=[sk], w=['wkv_b'])

                ck(3)
                qT_all = sb("qT_all", [64, L], BF16)
                kT_all = sb("kT_all", [64, L], BF16)
                va_all = sb("va_all", [128, NT, 65], BF16)
                V(lambda e: e.memset(va_all[:], 1.0), w=['va_all'])
                kmT = sb("kmT", [64, 32])
                kmp = sb("kmp", [64, 1])
                V(lambda e: e.memset(kmT[:], 0.0), w=['kmT'])
                mkT = sb("mkT", [64, 256], BF16)
                mva = sb("mva", [128, 2, 65], BF16)
                V(lambda e: e.memset(mva[:], 1.0), w=['mva'])
                ST = sb("ST", [128, 2, 64])
                STb = sb("STb", [128, 2, 64], BF16)
                V(lambda e: e.memset(ST[:], 0.0), w=['ST'])
                V(lambda e: e.memset(STb[:], 0.0), w=['STb'])
                pre = sb("pre", [128, 3, 131])
                V(lambda e: e.memset(pre[:], 0.0), w=['pre'])

                xt = [sb("xt%d" % i, [128, D]) for i in range(2)]
                sq = sb("sq", [128, D])
                ss = sb("ss", [128, 4])
                xn = sb("xn", [128, D], BF16)
                xnT = sb("xnT", [128, 8, 128], BF16)
                cv = sb("cv", [128, 3, 128])
                xs_f = sb("xs_f", [128, 128])
                BT = sb("BT", [128, 128], BF16)
                CT = sb("CT", [128, 128], BF16)
                x_tok = sb("x_tok", [128, 128])
                B_tok = sb("B_tok", [128, 128], BF16)
                zs = sb("zs", [128, 128])
                dtt = sb("dtt", [128, 8])
                sm6 = sb("sm6", [128, 6])
                E6 = sb("E6", [128, 6])
                GTm = sb("GTm", [128, 128])
                Ua = sb("Ua", [128, 128])
                Lm = sb("Lm", [128, 128])
                MT = sb("MT", [128, 128], BF16)
                xdt = sb("xdt", [128, 2, 64], BF16)
                xdte = sb("xdte", [128, 2, 64], BF16)
                y1 = sb("y1", [128, 64])
                qk = sb("qk", [128, 3, 64])
                qksq = sb("qksq", [128, 3, 64])
                qkn = sb("qkn", [128, 3, 64])
                qkss = sb("qkss", [128, 3])
                qTf = sb("qTf", [64, 128])
                xqT = sb("xqT", [64, 128], BF16)
                gm = sb("gm", [128, 32])
                pm = sb("pm", [128, 32])
                g8 = sb("g8", [128, 8])
                thr = sb("thr", [128, 1])
                nm = sb("nm", [128, 32])
                nmT = sb("nmT", [32, 256], BF16)
                pT = sb("pT", [128, 256], BF16)
                pxT = sb("pxT", [128, 256], BF16)
                rc = sb("rc", [128, 4])
                mixed = [sb("mixed%d" % i, [128, 256]) for i in range(4)]

                def norm_tile(src, skey, wtile, wkey):
                    A(lambda e: e.activation(out=sq[:], in_=src, func=AF.Square, accum_out=ss[:, 0:1]),
                      r=[skey], w=['sq', 'ss'])
                    rstd_inplace(ss[:, 0:1], D, 'ss')
                    V(lambda e: e.scalar_tensor_tensor(out=xn[:], in0=src, scalar=ss[:, 0:1], in1=wtile[:],
                                                       op0=ALU.mult, op1=ALU.mult), r=[skey, 'ss', wkey], w=['xn'])
                    for k in range(8):
                        P(lambda e: e.transpose(out=trp[:, k * 128:(k + 1) * 128], in_=xn[:, k * 128:(k + 1) * 128],
                                                identity=idb[:]), r=['xn', 'idb'], w=['trp'])
                    A(lambda e: e.copy(out=xnT[:].rearrange("p a b -> p (a b)"), in_=trp), r=['trp'], w=['xnT'])

                for mc in range(2):
                    DMA(xt[mc][:], mem[mc * 128:(mc + 1) * 128, :], 'xt%d' % mc, w=['xt%d' % mc])
                    norm_tile(xt[mc][:], 'xt%d' % mc, memwb, 'memwb')
                    for k in range(8):
                        P(lambda e: e.matmul(tm[:, 0:128], lhsT=xnT[:, k, :], rhs=wkv_b[:, k, :], start=(k == 0),
                                             stop=(k == 7)), r=['xnT', 'wkv_b'], w=['tm'])
                    A(lambda e: e.activation(out=qksq[:, 0, :], in_=tm[:, 0:64], func=AF.Square, accum_out=qkss[:, 0:1]),
                      r=['tm'], w=['qksq', 'qkss'])
                    rstd_inplace(qkss[:, 0:1], 64, 'qkss')
                    V(lambda e: e.scalar_tensor_tensor(out=qkn[:, 0, :], in0=tm[:, 0:64], scalar=qkss[:, 0:1],
                                                       in1=xkwb[:], op0=ALU.mult, op1=ALU.mult),
                      r=['tm', 'qkss', 'xkwb'], w=['qkn'])
                    P(lambda e: e.transpose(out=p_kT, in_=qkn[:, 0, :], identity=idf[:]), r=['qkn', 'idf'], w=['p_kT'])
                    A(lambda e: e.copy(out=mkT[:, mc * 128:(mc + 1) * 128], in_=p_kT), r=['p_kT'], w=['mkT'])
                    A(lambda e: e.copy(out=mva[:, mc, 0:64], in_=tm[:, 64:128]), r=['tm'], w=['mva'])

                ck(4)
                def load_x(i):
                    k = 'xt%d' % (i % 2)
                    DMA(xt[i % 2][:], x_full[i * 128:(i + 1) * 128, :], k, w=[k])

                load_x(0)
                for i in range(nt1):
                    if i + 1 < nt1:
                        load_x(i + 1)
                    xk = 'xt%d' % (i % 2)
                    mx = mixed[i % 4]
                    mxk = 'mixed%d' % (i % 4)
                    bq = i // 2
                    norm_tile(xt[i % 2][:], xk, wb, 'wb')
                    ckl(i, 41)
                    for blk in range(3):
                        for k in range(8):
                            P(lambda e: e.matmul(fm[blk], lhsT=w_in_b[:, k, blk * 128:(blk + 1) * 128], rhs=xnT[:, k, :],
                                                 start=(k == 0), stop=(k == 7)), r=['w_in_b', 'xnT'], w=['fm'])
                    for k in range(8):
                        P(lambda e: e.matmul(tm, lhsT=xnT[:, k, :], rhs=w_in_b[:, k, 384:770], start=(k == 0),
                                             stop=(k == 7)), r=['xnT', 'w_in_b'], w=['tm'])
                    A(lambda e: e.copy(out=pre[:, :, 3:131], in_=pb_fm[:, 0:384].rearrange("p (a b) -> p a b", b=128)),
                      r=['fm'], w=['pre'])
                    ckl(i, 42)
                    for blk in range(3):
                        V(lambda e: e.tensor_scalar(out=cv[:, blk, :], in0=pre[:, blk, 0:128],
                                                    scalar1=cw[:, blk * 4:blk * 4 + 1], scalar2=cb[:, blk:blk + 1],
                                                    op0=ALU.mult, op1=ALU.add), r=['pre', 'cw', 'cb'], w=['cv'])
                        for t in range(1, 4):
                            V(lambda e: e.scalar_tensor_tensor(out=cv[:, blk, :], in0=pre[:, blk, t:t + 128],
                                                               scalar=cw[:, blk * 4 + t:blk * 4 + t + 1],
                                                               in1=cv[:, blk, :], op0=ALU.mult, op1=ALU.add),
                              r=['pre', 'cw', 'cv'], w=['cv'])
                    V(lambda e: e.tensor_copy(out=pre[:, :, 0:3], in_=pre[:, :, 128:131]), r=['pre'], w=['pre'])
                    A(lambda e: e.activation(out=xs_f[:], in_=cv[:, 0, :], func=AF.Silu), r=['cv'], w=['xs_f'])
                    A(lambda e: e.activation(out=BT[:], in_=cv[:, 1, :], func=AF.Silu), r=['cv'], w=['BT'])
                    A(lambda e: e.activation(out=CT[:], in_=cv[:, 2, :], func=AF.Silu), r=['cv'], w=['CT'])
                    A(lambda e: e.activation(out=zs[:], in_=tm[:, 0:128], func=AF.Silu), r=['tm'], w=['zs'])
                    ckl(i, 43)
                    P(lambda e: e.transpose(out=p_xt, in_=xs_f[:], identity=idf[:]), r=['xs_f', 'idf'], w=['p_xt'])
                    P(lambda e: e.transpose(out=p_bt, in_=BT[:], identity=idb[:]), r=['BT', 'idb'], w=['p_bt'])
                    A(lambda e: e.copy(out=x_tok[:], in_=p_xt), r=['p_xt'], w=['x_tok'])
                    A(lambda e: e.copy(out=B_tok[:], in_=p_bt), r=['p_bt'], w=['B_tok'])
                    ckl(i, 44)
                    V(lambda e: e.tensor_tensor(out=dtt[:, 0:2], in0=tm[:, 128:130], in1=sv[:, 0:2], op=ALU.add),
                      r=['tm', 'sv'], w=['dtt'])
                    ckl(i, 441)
                    A(lambda e: e.activation(out=dtt[:, 0:2], in_=dtt[:, 0:2], func=AF.Exp), r=['dtt'], w=['dtt'])
                    ckl(i, 442)
                    A(lambda e: e.activation(out=dtt[:, 2:4], in_=dtt[:, 0:2], func=AF.Ln, bias=1.0), r=['dtt'], w=['dtt'])
                    V(lambda e: e.tensor_tensor(out=dtt[:, 4:6], in0=dtt[:, 2:4], in1=acoef[:], op=ALU.mult),
                      r=['dtt', 'acoef'], w=['dtt'])
                    ckl(i, 443)
                    P(lambda e: e.matmul(p_acs[:, 0:2], lhsT=Um[:], rhs=dtt[:, 4:6], start=True, stop=True),
                      r=['Um', 'dtt'], w=['p_acs'])
                    P(lambda e: e.matmul(p_acs[:, 2:4], lhsT=ones[:], rhs=dtt[:, 4:6], start=True, stop=True),
                      r=['ones', 'dtt'], w=['p_acs'])
                    ckl(i, 444)
                    V(lambda e: e.tensor_copy(out=sm6[:, 0:4], in_=p_acs), r=['p_acs'], w=['sm6'])
                    V(lambda e: e.tensor_tensor(out=sm6[:, 4:6], in0=sm6[:, 2:4], in1=sm6[:, 0:2], op=ALU.subtract),
                      r=['sm6'], w=['sm6'])
                    ckl(i, 445)
                    A(lambda e: e.activation(out=E6[:], in_=sm6[:], func=AF.Exp), r=['sm6'], w=['E6'])
                    V(lambda e: e.tensor_tensor(out=dtt[:, 6:8], in0=dtt[:, 2:4], in1=E6[:, 4:6], op=ALU.mult),
                      r=['dtt', 'E6'], w=['dtt'])
                    ckl(i, 45)
                    P(lambda e: e.matmul(p_gt, lhsT=BT[:], rhs=CT[:], start=True, stop=True), r=['BT', 'CT'], w=['p_gt'])
                    V(lambda e: e.tensor_tensor(out=GTm[:], in0=p_gt, in1=Um[:], op=ALU.mult), r=['p_gt', 'Um'], w=['GTm'])
                    ckl(i, 46)
                    for h in range(2):
                        hs = slice(h * 64, (h + 1) * 64)
                        if h == 0: ckl(i, 461)
                        V(lambda e: e.tensor_scalar(out=Ua[:], in0=Um[:], scalar1=dtt[:, 4 + h:5 + h], scalar2=None,
                                                    op0=ALU.mult), r=['Um', 'dtt'], w=['Ua'])
                        if h == 0: ckl(i, 462)
                        P(lambda e: e.matmul(p_dt[h], lhsT=Ls[:], rhs=Ua[:], start=True, stop=True),
                          r=['Ls', 'Ua'], w=['p_dt%d' % h])
                        if h == 0: ckl(i, 463)
                        A(lambda e: e.activation(out=Lm[:], in_=p_dt[h], func=AF.Exp), r=['p_dt%d' % h], w=['Lm'])
                        if h == 0: ckl(i, 464)
                        V(lambda e: e.tensor_tensor(out=MT[:], in0=Lm[:], in1=GTm[:], op=ALU.mult),
                          r=['Lm', 'GTm'], w=['MT'])
                        if h == 0: ckl(i, 465)
                        V(lambda e: e.tensor_scalar(out=xdt[:, h, :], in0=x_tok[:, hs], scalar1=dtt[:, 2 + h:3 + h],
                                                    scalar2=None, op0=ALU.mult), r=['x_tok', 'dtt'], w=['xdt%d' % h])
                        if h == 0: ckl(i, 466)
                        V(lambda e: e.tensor_scalar(out=xdte[:, h, :], in0=x_tok[:, hs], scalar1=dtt[:, 6 + h:7 + h],
                                                    scalar2=None, op0=ALU.mult), r=['x_tok', 'dtt'], w=['xdte%d' % h])
                        if h == 0: ckl(i, 467)
                        P(lambda e: e.matmul(p_y[h], lhsT=MT[:], rhs=xdt[:, h, :], start=True, stop=True),
                          r=['MT', 'xdt%d' % h], w=['p_y%d' % h])
                        if h == 0: ckl(i, 468)
                        P(lambda e: e.matmul(p_yo[h], lhsT=CT[:], rhs=STb[:, h, :], start=True, stop=True),
                          r=['CT', 'STb%d' % h], w=['p_yo%d' % h])
                        if h == 0: ckl(i, 469)
                        P(lambda e: e.matmul(p_sn[h], lhsT=B_tok[:], rhs=xdte[:, h, :], start=True, stop=True),
                          r=['B_tok', 'xdte%d' % h], w=['p_sn%d' % h])
                        if h == 0: ckl(i, 470)
                        V(lambda e: e.tensor_copy(out=y1[:], in_=p_y[h]), r=['p_y%d' % h], w=['y1'])
                        if h == 0: ckl(i, 471)
                        V(lambda e: e.scalar_tensor_tensor(out=y1[:], in0=p_yo[h], scalar=E6[:, h:h + 1], in1=y1[:],
                                                           op0=ALU.mult, op1=ALU.add),
                          r=['p_yo%d' % h, 'E6', 'y1'], w=['y1'])
                        if h == 0: ckl(i, 472)
                        V(lambda e: e.scalar_tensor_tensor(out=y1[:], in0=x_tok[:, hs], scalar=sv[:, 4 + h:5 + h],
                                                           in1=y1[:], op0=ALU.mult, op1=ALU.add),
                          r=['x_tok', 'sv', 'y1'], w=['y1'])
                        if h == 0: ckl(i, 473)
                        V(lambda e: e.tensor_tensor(out=mx[:, hs], in0=y1[:], in1=zs[:, hs], op=ALU.mult),
                          r=['y1', 'zs'], w=[mxk])
                        if h == 0: ckl(i, 474)
                        V(lambda e: e.scalar_tensor_tensor(out=ST[:, h, :], in0=ST[:, h, :], scalar=E6[:, 2 + h:3 + h],
                                                           in1=p_sn[h], op0=ALU.mult, op1=ALU.add),
                          r=['ST%d' % h, 'E6', 'p_sn%d' % h], w=['ST%d' % h])
                        if h == 0: ckl(i, 475)
                        V(lambda e: e.tensor_copy(out=STb[:, h, :], in_=ST[:, h, :]), r=['ST%d' % h], w=['STb%d' % h])

                    ckl(i, 47)
                    A(lambda e: e.copy(out=qk[:].rearrange("p a b -> p (a b)"), in_=tm[:, 130:322]), r=['tm'], w=['qk'])
                    A(lambda e: e.copy(out=va_all[:, i, 0:64], in_=tm[:, 322:386]), r=['tm'], w=[('va', i)])
                    A(lambda e: e.activation(out=qksq[:], in_=qk[:], func=AF.Square), r=['qk'], w=['qksq'])
                    V(lambda e: e.tensor_reduce(out=qkss[:], in_=qksq[:], axis=AX.X, op=ALU.add), r=['qksq'], w=['qkss'])
                    rstd_inplace(qkss[:], 64, 'qkss')
                    V(lambda e: e.tensor_tensor(out=qkn[:], in0=qk[:], in1=qkss[:].unsqueeze(2).to_broadcast([128, 3, 64]),
                                                op=ALU.mult), r=['qk', 'qkss'], w=['qkn'])
                    V(lambda e: e.tensor_tensor(out=qkn[:], in0=qkn[:], in1=qkwb[:].rearrange("p (a b) -> p a b", b=64),
                                                op=ALU.mult), r=['qkn', 'qkwb'], w=['qkn'])
                    ts = slice(i * 128, (i + 1) * 128)
                    P(lambda e: e.transpose(out=p_qT, in_=qkn[:, 0, :], identity=idf[:]), r=['qkn', 'idf'], w=['p_qT'])
                    P(lambda e: e.transpose(out=p_kT, in_=qkn[:, 1, :], identity=idf[:]), r=['qkn', 'idf'], w=['p_kT'])
                    P(lambda e: e.transpose(out=p_xqT, in_=qkn[:, 2, :], identity=idf[:]), r=['qkn', 'idf'], w=['p_xqT'])
                    A(lambda e: e.copy(out=qT_all[:, ts], in_=p_qT), r=['p_qT'], w=[('qT', i)])
                    A(lambda e: e.copy(out=qTf[:], in_=p_qT), r=['p_qT'], w=['qTf'])
                    A(lambda e: e.copy(out=kT_all[:, ts], in_=p_kT), r=['p_kT'], w=[('kT', i)])
                    A(lambda e: e.copy(out=xqT[:], in_=p_xqT), r=['p_xqT'], w=['xqT'])
                    P(lambda e: e.matmul(p_km, lhsT=qkn[:, 1, :], rhs=onesc[:], start=True, stop=True),
                      r=['qkn', 'onesc'], w=['p_km'])
                    if i % 2 == 0:
                        V(lambda e: e.tensor_copy(out=kmp[:], in_=p_km), r=['p_km'], w=['kmp'])
                    P(lambda e: e.matmul(p_gate, lhsT=qTf[:], rhs=kmT[:], start=True, stop=True),
                      r=['qTf', 'kmT'], w=['p_gate'])
                    V(lambda e: e.tensor_scalar(out=pm[:], in0=iof[:, 0:32], scalar1=float(bq), scalar2=-1e30,
                                                op0=ALU.is_ge, op1=ALU.mult), r=['iof'], w=['pm'])
                    V(lambda e: e.tensor_tensor(out=gm[:], in0=p_gate, in1=pm[:], op=ALU.add), r=['p_gate', 'pm'], w=['gm'])
                    V(lambda e: e.max(out=g8[:], in_=gm[:]), r=['gm'], w=['g8'])
                    V(lambda e: e.tensor_scalar(out=thr[:], in0=g8[:, 2:3], scalar1=-1e29, scalar2=None, op0=ALU.max),
                      r=['g8'], w=['thr'])
                    V(lambda e: e.tensor_scalar(out=nm[:], in0=gm[:], scalar1=thr[:, 0:1], scalar2=-1.0, op0=ALU.is_ge,
                                                op1=ALU.add), r=['gm', 'thr'], w=['nm'])
                    P(lambda e: e.transpose(out=p_nmT, in_=nm[:], identity=idf[:]), r=['nm', 'idf'], w=['p_nmT'])
                    A(lambda e: e.copy(out=nmT[:, (i % 2) * 128:(i % 2 + 1) * 128], in_=p_nmT), r=['p_nmT'], w=['nmT'])
                    if i % 2 == 1:
                        V(lambda e: e.tensor_tensor(out=kmT[:, bq:bq + 1], in0=kmp[:], in1=p_km, op=ALU.add), r=['p_km', 'kmp'], w=['kmT'])

                    ckl(i, 48)
                    for mc in range(2):
                        P(lambda e: e.matmul(p_xs[:, mc * 128:(mc + 1) * 128], lhsT=mkT[:, mc * 128:(mc + 1) * 128],
                                             rhs=xqT[:], start=True, stop=True), r=['mkT', 'xqT'], w=['p_xs'])
                    A(lambda e: e.activation(out=pxT[:], in_=p_xs, func=AF.Exp, scale=0.125), r=['p_xs'], w=['pxT'])
                    for mc in range(2):
                        P(lambda e: e.matmul(p_ox, lhsT=pxT[:, mc * 128:(mc + 1) * 128], rhs=mva[:, mc, :],
                                             start=(mc == 0), stop=(mc == 1)), r=['pxT', 'mva'], w=['p_ox'])
                    V(lambda e: e.reciprocal(out=rc[:, 0:1], in_=p_ox[:, 64:65]), r=['p_ox'], w=['rc'])
                    V(lambda e: e.tensor_scalar(out=mx[:, 192:256], in0=p_ox[:, 0:64], scalar1=rc[:, 0:1], scalar2=None,
                                                op0=ALU.mult), r=['p_ox', 'rc'], w=[mxk])

                    ckl(i, 49)
                    if debug and i == 1:
                        dq = sb("dq", [128, 1024])
                        V(lambda e: e.memset(dq[:], 0.0), w=['dq'])
                        V(lambda e: e.tensor_copy(out=dq[0:64, 0:256], in_=qT_all[:, 0:256]), r=[('qT', 0), ('qT', 1)], w=['dq'])
                        V(lambda e: e.tensor_copy(out=dq[0:64, 256:512], in_=kT_all[:, 0:256]), r=[('kT', 0), ('kT', 1)], w=['dq'])
                        V(lambda e: e.tensor_copy(out=dq[:, 512:768], in_=cm0[:]), r=['cm0'], w=['dq'])
                        V(lambda e: e.tensor_copy(out=dq[:, 768:1024], in_=cm1[:]), r=['cm1'], w=['dq'])
                        DMA(dbg_q, dq[:], 'dbgq', r=['dq'], w=['dbg_q'])
                    if i % 2 == 1:
                        qs = slice(bq * 256, (bq + 1) * 256)
                        nks = 2 * bq + 2
                        for ks in range(nks):
                            own = ks >= 2 * bq
                            P(lambda e: e.matmul(p_sc, lhsT=kT_all[:, ks * 128:(ks + 1) * 128], rhs=qT_all[:, qs],
                                                 start=True, stop=False),
                              r=[('kT', ks), ('qT', i - 1), ('qT', i)], w=['p_sc'])
                            if not own:
                                P(lambda e: e.matmul(p_sc, lhsT=sel30[:, ks // 2, :], rhs=nmT[:], start=False, stop=True),
                                  r=['sel30', 'nmT'], w=['p_sc'])
                            else:
                                cmx = cm0 if ks == 2 * bq else cm1
                                P(lambda e: e.matmul(p_sc, lhsT=i30[:], rhs=cmx[:], start=False, stop=True),
                                  r=['i30', 'cm0', 'cm1'], w=['p_sc'])
                            m = 2 * bq + 1 - ks
                            A(lambda e: e.activation(out=pT[:], in_=p_sc, func=AF.Exp, bias=alib[:, m:m + 1], scale=0.125),
                              r=['p_sc', 'alib'], w=['pT'])
                            for qt in range(2):
                                P(lambda e: e.matmul(p_om[qt], lhsT=pT[:, qt * 128:(qt + 1) * 128], rhs=va_all[:, ks, :],
                                                     start=(ks == 0), stop=(ks == nks - 1)),
                                  r=['pT', ('va', ks)], w=['p_om%d' % qt])
                        for qt in range(2):
                            j = i - 1 + qt
                            V(lambda e: e.reciprocal(out=rc[:, 1 + qt:2 + qt], in_=p_om[qt][:, 64:65]),
                              r=['p_om%d' % qt], w=['rc'])
                            V(lambda e: e.tensor_scalar(out=mixed[j % 4][:, 128:192], in0=p_om[qt][:, 0:64],
                                                        scalar1=rc[:, 1 + qt:2 + qt], scalar2=None, op0=ALU.mult),
                              r=['p_om%d' % qt, 'rc'], w=['mixed%d' % (j % 4)])
                            DMA(ag_in.ap()[j * 128:(j + 1) * 128, :], mixed[j % 4][:], 'agw%d' % (j % 4),
                                r=['mixed%d' % (j % 4)], w=[('agin', j)])
                            if debug:
                                DMA(dbg_mixed[j * 128:(j + 1) * 128, :], mixed[j % 4][:], 'dbgm%d' % (j % 4),
                                    r=['mixed%d' % (j % 4)], w=['dbg_mixed'])

                ck(5)

                mk._deps('pool', [('agin', j) for j in range(nt1)], ['agout'])
                ccs = es0.enter_context(nc.semaphore('ccs'))
                ev = ('cc', ccs, 8, 'dma')
                if not mk.stopped:
                    for ci in range(8):
                        nc.gpsimd.collective_compute(
                            "AllGather", ALU.bypass, replica_groups=[[0, 1, 2, 3], [4, 5, 6, 7]],
                            ins=[ag_in.ap()[ci * 1024:(ci + 1) * 1024, :]],
                            outs=[ag_out.ap()[ci * 4096:(ci + 1) * 4096, :]]).then_inc(ccs, 1)
                    mk._commit(ev, [('agin', j) for j in range(nt1)], ['agout'])
                for e in ('pe', 'act', 'dve', 'sp'):
                    mk._wait(e, ev)
                for e in ('pe', 'act', 'dve', 'pool'):
                    for e2 in ('pe', 'act', 'dve', 'pool'):
                        if e != e2 and mk.cnt[e2] > 0:
                            mk._wait(e, ('e_' + e2, mk.sem[e2], mk.cnt[e2], e2))

            ck(6)
            with ExitStack() as es2:
                def sb(name, shape, dt=F32):
                    return es2.enter_context(nc.sbuf_tensor(name, list(shape), dt))

                def psb(name):
                    return es2.enter_context(nc.psum_tensor(name, [128, 512], F32))

                pb_op = [psb("pb_op0"), psb("pb_op1")]
                pb_ta = [psb("pb_ta0"), psb("pb_ta1")]
                pb_s = [psb("pb_s%d" % i) for i in range(4)]
                trp2 = pb_s[0][:, 0:512].bitcast(BF16)

                w_out_b = sb("w_out_b", [128, 8, D], BF16)
                wq_f = sb("wq_f", [128, 8, D])
                wst2 = [sb("wst2_%d" % i, [128, D]) for i in range(2)]
                for k in range(8):
                    s = wst2[k % 2]
                    sk = 'wst2_%d' % (k % 2)
                    DMA(s[:], w_out[k * 128:(k + 1) * 128, :], sk, w=[sk])
                    A(lambda e: e.copy(out=w_out_b[:, k, :], in_=s[:]), r=[sk], w=['w_out_b'])
                DMA(wq_f[:], wq.rearrange("(k p) c -> p k c", p=128), 'wq_f', w=['wq_f'])
                kkf = sb("kkf", [128, 256])
                DMA(kkf[:], kk, 'kkf', w=['kkf'])
                ffnwb = sb("ffnwb", [128, D])
                DMA(ffnwb[:], ffnw.partition_broadcast(128), 'ffnwb', w=['ffnwb'])
                snwb = sb("snwb", [128, 512])
                DMA(snwb[:], ssdnw.partition_broadcast(128), 'snwb', w=['snwb'])
                rix = sb("rix", [128, NT2 * 4], I32)
                DMA(rix[:], ridx, 'rix', w=['rix'])

                ck(7)
                mg = sb("mg", [128, 4, 256])
                sq2 = sb("sq2", [128, D])
                ss2 = sb("ss2", [128, 4])
                mixn = sb("mixn", [128, 4, 256], BF16)
                mixT = sb("mixT", [128, 8, 128], BF16)
                xr = sb("xr", [128, D])
                hh = [sb("hh%d" % i, [128, D]) for i in range(2)]
                hn = sb("hn", [128, D])
                hnT = sb("hnT", [128, 8, 128])
                qTh = sb("qTh", [128, 8, 128])
                sc = sb("sc", [128, 16, 128])
                scw = sb("scw", [128, 128])
                v12 = sb("v12", [128, 16, 16])
                i12 = sb("i12", [128, 16, 16], U32)
                i12f = sb("i12f", [128, 16, 16])
                cand = sb("cand", [128, 8, 256])
                candw = sb("candw", [128, 256])
                tops = sb("tops", [128, 8, 16])
                pos = sb("pos", [128, 8, 16], U32)
                pa = sb("pa", [128, 8, 16], U32)
                pbb = sb("pbb", [128, 8, 16], U32)
                paf = sb("paf", [128, 128])
                pbf = sb("pbf", [128, 128])
                oh = sb("oh", [128, 128, 16])
                i1s = sb("i1s", [128, 128])
                i2s = sb("i2s", [128, 128])
                ef = sb("ef", [128, 128])
                ei = sb("ei", [128, 128], I32)
                gte = sb("gte", [128, 8, 16])
                gsum = sb("gsum", [128, 8])
                uu = sb("uu", [128, 128])
                ga = sb("ga", [128, 128])
                gbuf = [sb("gbuf%d" % i, [128, D]) for i in range(NBUF)]
                scr = sb("scr", [128, D])

                for tt in range(nt2):
                    hcur = hh[tt % 2]
                    hk = 'hh%d' % (tt % 2)
                    for r in range(4):
                        mk.dma('pool', lambda e: e.indirect_dma_start(
                            out=mg[:, r, :], out_offset=None, in_=ag_out.ap(),
                            in_offset=bass.IndirectOffsetOnAxis(ap=rix[:, tt * 4 + r:tt * 4 + r + 1], axis=0)),
                            'mg%d' % r, reads=['rix', 'agout'], writes=['mg'])
                    DMA(xr[:], x_res[tt * 128:(tt + 1) * 128, :], 'xr', w=['xr'])
                    for grp in range(2):
                        A(lambda e: e.activation(out=sq2[:, 0:256].rearrange("p (a b) -> p a b", b=128),
                                                 in_=mg[:, 2 * grp:2 * grp + 2, 0:128], func=AF.Square,
                                                 accum_out=ss2[:, grp:grp + 1]), r=['mg'], w=['sq2', 'ss2'])
                    rstd_inplace(ss2[:, 0:2], 256, 'ss2')
                    for r in range(4):
                        V(lambda e: e.scalar_tensor_tensor(out=mixn[:, r, 0:128], in0=mg[:, r, 0:128],
                                                           scalar=ss2[:, r // 2:r // 2 + 1],
                                                           in1=snwb[:, r * 128:(r + 1) * 128], op0=ALU.mult, op1=ALU.mult),
                          r=['mg', 'ss2', 'snwb'], w=['mixn'])
                    A(lambda e: e.copy(out=mixn[:, :, 128:256], in_=mg[:, :, 128:256]), r=['mg'], w=['mixn'])
                    mixn2 = mixn[:].rearrange("p a b -> p (a b)")
                    for k in range(8):
                        P(lambda e: e.transpose(out=trp2[:, k * 128:(k + 1) * 128], in_=mixn2[:, k * 128:(k + 1) * 128],
                                                identity=idb[:]), r=['mixn', 'idb'], w=['pb_s0'])
                    A(lambda e: e.copy(out=mixT[:].rearrange("p a b -> p (a b)"), in_=trp2), r=['pb_s0'], w=['mixT'])
                    for half in range(2):
                        for k in range(8):
                            P(lambda e: e.matmul(pb_op[half][:, :], lhsT=mixT[:, k, :],
                                                 rhs=w_out_b[:, k, half * 512:(half + 1) * 512], start=(k == 0),
                                                 stop=(k == 7)), r=['mixT', 'w_out_b'], w=['pb_op%d' % half])
                        V(lambda e: e.tensor_tensor(out=hcur[:, half * 512:(half + 1) * 512], in0=pb_op[half][:, :],
                                                    in1=xr[:, half * 512:(half + 1) * 512], op=ALU.add),
                          r=['pb_op%d' % half, 'xr'], w=[hk])
                    if debug:
                        DMA(dbg_h[tt * 128:(tt + 1) * 128, :], hcur[:], 'dbgh', r=[hk], w=['dbg_h'])
                    A(lambda e: e.activation(out=sq2[:], in_=hcur[:], func=AF.Square, accum_out=ss2[:, 2:3]),
                      r=[hk], w=['sq2', 'ss2b'])
                    rstd_inplace(ss2[:, 2:3], D, 'ss2b')
                    V(lambda e: e.scalar_tensor_tensor(out=hn[:], in0=hcur[:], scalar=ss2[:, 2:3], in1=ffnwb[:],
                                                       op0=ALU.mult, op1=ALU.mult), r=[hk, 'ss2b', 'ffnwb'], w=['hn'])
                    for k in range(8):
                        P(lambda e: e.transpose(out=pb_ta[k // 4][:, (k % 4) * 128:(k % 4 + 1) * 128],
                                                in_=hn[:, k * 128:(k + 1) * 128], identity=idf[:]),
                          r=['hn', 'idf'], w=['pb_ta%d' % (k // 4)])
                    for j in range(2):
                        A(lambda e: e.copy(out=hnT[:, j * 4:(j + 1) * 4, :].rearrange("p a b -> p (a b)"),
                                           in_=pb_ta[j][:, :]), r=['pb_ta%d' % j], w=['hnT'])
                    for h in range(8):
                        for k in range(8):
                            P(lambda e: e.matmul(pb_ta[h // 4][:, (h % 4) * 128:(h % 4 + 1) * 128],
                                                 lhsT=wq_f[:, k, h * 128:(h + 1) * 128], rhs=hnT[:, k, :],
                                                 start=(k == 0), stop=(k == 7)),
                              r=['wq_f', 'hnT'], w=['pb_ta%d' % (h // 4)])
                    for j in range(2):
                        A(lambda e: e.copy(out=qTh[:, j * 4:(j + 1) * 4, :].rearrange("p a b -> p (a b)"),
                                           in_=pb_ta[j][:, :]), r=['pb_ta%d' % j], w=['qTh'])
                    for h in range(8):
                        P(lambda e: e.matmul(pb_s[h // 2][:, (h % 2) * 256:(h % 2 + 1) * 256], lhsT=qTh[:, h, :],
                                             rhs=kkf[:], start=True, stop=True), r=['qTh', 'kkf'], w=['pb_s%d' % (h // 2)])
                    for j in range(4):
                        A(lambda e: e.copy(out=sc[:, j * 4:(j + 1) * 4, :].rearrange("p a b -> p (a b)"),
                                           in_=pb_s[j][:, :]), r=['pb_s%d' % j], w=['sc'])
                    for c in range(16):
                        V(lambda e: e.max(out=v12[:, c, 0:8], in_=sc[:, c, :]), r=['sc'], w=['v12'])
                        V(lambda e: e.max_index(out=i12[:, c, 0:8], in_max=v12[:, c, 0:8], in_values=sc[:, c, :]),
                          r=['sc', 'v12'], w=['i12'])
                        V(lambda e: e.match_replace(out=scw[:], in_to_replace=v12[:, c, 0:8], in_values=sc[:, c, :],
                                                    imm_value=-1e30), r=['sc', 'v12'], w=['scw'])
                        V(lambda e: e.max(out=v12[:, c, 8:16], in_=scw[:]), r=['scw'], w=['v12'])
                        V(lambda e: e.max_index(out=i12[:, c, 8:16], in_max=v12[:, c, 8:16], in_values=scw[:]),
                          r=['scw', 'v12'], w=['i12'])
                    V(lambda e: e.tensor_copy(out=i12f[:], in_=i12[:]), r=['i12'], w=['i12f'])
                    v4 = v12[:].rearrange("p (h t) k -> p h t k", t=2)
                    V(lambda e: e.tensor_tensor(out=cand[:].rearrange("p h (a b) -> p h a b", b=16),
                                                in0=v4[:, :, 0, :].unsqueeze(3).to_broadcast([128, 8, 16, 16]),
                                                in1=v4[:, :, 1, :].unsqueeze(2).to_broadcast([128, 8, 16, 16]),
                                                op=ALU.add), r=['v12'], w=['cand'])
                    for h in range(8):
                        V(lambda e: e.max(out=tops[:, h, 0:8], in_=cand[:, h, :]), r=['cand'], w=['tops'])
                        V(lambda e: e.max_index(out=pos[:, h, 0:8], in_max=tops[:, h, 0:8], in_values=cand[:, h, :]),
                          r=['cand', 'tops'], w=['pos'])
                        V(lambda e: e.match_replace(out=candw[:], in_to_replace=tops[:, h, 0:8], in_values=cand[:, h, :],
                                                    imm_value=-1e30), r=['cand', 'tops'], w=['candw'])
                        V(lambda e: e.max(out=tops[:, h, 8:16], in_=candw[:]), r=['candw'], w=['tops'])
                        V(lambda e: e.max_index(out=pos[:, h, 8:16], in_max=tops[:, h, 8:16], in_values=candw[:]),
                          r=['candw', 'tops'], w=['pos'])
                    V(lambda e: e.tensor_tensor(out=gte[:], in0=tops[:], in1=tops[:, :, 0:1].to_broadcast([128, 8, 16]),
                                                op=ALU.subtract), r=['tops'], w=['gte'])
                    A(lambda e: e.activation(out=gte[:], in_=gte[:], func=AF.Exp), r=['gte'], w=['gte'])
                    V(lambda e: e.tensor_reduce(out=gsum[:], in_=gte[:], axis=AX.X, op=ALU.add), r=['gte'], w=['gsum'])
                    V(lambda e: e.reciprocal(out=gsum[:], in_=gsum[:]), r=['gsum'], w=['gsum'])
                    V(lambda e: e.tensor_tensor(out=gte[:], in0=gte[:], in1=gsum[:].unsqueeze(2).to_broadcast([128, 8, 16]),
                                                op=ALU.mult), r=['gte', 'gsum'], w=['gte'])
                    V(lambda e: e.tensor_single_scalar(out=pa[:], in_=pos[:], scalar=4, op=ALU.logical_shift_right),
                      r=['pos'], w=['pa'])
                    V(lambda e: e.tensor_single_scalar(out=pbb[:], in_=pos[:], scalar=15, op=ALU.bitwise_and),
                      r=['pos'], w=['pbb'])
                    V(lambda e: e.tensor_copy(out=paf[:], in_=pa[:].rearrange("p h k -> p (h k)")), r=['pa'], w=['paf'])
                    V(lambda e: e.tensor_copy(out=pbf[:], in_=pbb[:].rearrange("p h k -> p (h k)")), r=['pbb'], w=['pbf'])
                    i4 = i12f[:].rearrange("p (h t) k -> p h t k", t=2)
                    for (pf, pfk, half, dst, dk) in ((paf, 'paf', 0, i1s, 'i1s'), (pbf, 'pbf', 1, i2s, 'i2s')):
                        V(lambda e: e.tensor_tensor(out=oh[:], in0=pf[:].unsqueeze(2).to_broadcast([128, 128, 16]),
                                                    in1=iof[:, 0:16].unsqueeze(1).to_broadcast([128, 128, 16]),
                                                    op=ALU.is_equal), r=[pfk, 'iof'], w=['oh'])
                        V(lambda e: e.tensor_tensor(out=oh[:].rearrange("p (h k) a -> p h k a", k=16),
                                                    in0=oh[:].rearrange("p (h k) a -> p h k a", k=16),
                                                    in1=i4[:, :, half, :].unsqueeze(2).to_broadcast([128, 8, 16, 16]),
                                                    op=ALU.mult), r=['oh', 'i12f'], w=['oh'])
                        V(lambda e: e.tensor_reduce(out=dst[:], in_=oh[:], axis=AX.X, op=ALU.add), r=['oh'], w=[dk])
                    V(lambda e: e.scalar_tensor_tensor(out=ef[:], in0=i1s[:], scalar=128.0, in1=i2s[:], op0=ALU.mult,
                                                       op1=ALU.add), r=['i1s', 'i2s'], w=['ef'])
                    V(lambda e: e.tensor_copy(out=ei[:], in_=ef[:]), r=['ef'], w=['ei'])
                    V(lambda e: e.memset(uu[:], 0.0), w=['uu'])
                    nb = 0
                    for s in range(0 if noexp else 128):
                        b_ = (tt * 256 + s) % NBUF
                        bk = 'gbuf%d' % b_
                        mk.dma('pool', lambda e: e.indirect_dma_start(
                            out=gbuf[b_][:], out_offset=None, in_=e_dn,
                            in_offset=bass.IndirectOffsetOnAxis(ap=ei[:, s:s + 1], axis=0)),
                            bk, reads=['ei'], writes=[bk])
                        V(lambda e: e.scalar_tensor_tensor(out=scr[:], in0=gbuf[b_][:], scalar=1.0, in1=hn[:],
                                                           op0=ALU.mult, op1=ALU.mult, accum_out=uu[:, s:s + 1]),
                          r=[bk, 'hn'], w=['scr', 'uu'])
                    A(lambda e: e.activation(out=ga[:], in_=uu[:], func=AF.Gelu), r=['uu'], w=['ga'])
                    V(lambda e: e.tensor_tensor(out=ga[:], in0=ga[:], in1=gte[:].rearrange("p h k -> p (h k)"),
                                                op=ALU.mult), r=['ga', 'gte'], w=['ga'])
                    for s in range(0 if noexp else 128):
                        b_ = (tt * 256 + 128 + s) % NBUF
                        bk = 'gbuf%d' % b_
                        mk.dma('pool', lambda e: e.indirect_dma_start(
                            out=gbuf[b_][:], out_offset=None, in_=e_up,
                            in_offset=bass.IndirectOffsetOnAxis(ap=ei[:, s:s + 1], axis=0)),
                            bk, reads=['ei'], writes=[bk])
                        V(lambda e: e.scalar_tensor_tensor(out=hcur[:], in0=gbuf[b_][:], scalar=ga[:, s:s + 1],
                                                           in1=hcur[:], op0=ALU.mult, op1=ALU.add),
                          r=[bk, 'ga', hk], w=[hk])
                    DMA(out[tt * 128:(tt + 1) * 128, :], hcur[:], 'out%d' % (tt % 2), r=[hk], w=[('out', tt)])

        except _Stop:
            pass
        mk.stopped = False
        keys = [k for k in [('out', tt) for tt in range(nt2)] + ['dbg_h', 'dbg_mixed'] if k in mk.res]
        mk.finish('sp', keys)
        for cn, c in mk.chan.items():
            if c[1] > 0:
                mk._wait('sp', ('c_' + str(cn), c[0], c[1], 'dma'))
        for e in ('pe', 'act', 'dve', 'pool'):
            if mk.cnt[e] > 0:
                mk._wait('sp', ('e_' + e, mk.sem[e], mk.cnt[e], e))
    return nc


def prep_inputs(x, mem, mix_norm_w, w_in, ssd_conv_w, ssd_conv_b, ssd_dt_bias, ssd_a_log, ssd_d,
                ssd_norm_w, moba_q_norm_w, moba_k_norm_w, mem_norm_w, w_mem_kv, xattn_q_norm_w,
                xattn_k_norm_w, w_out, ffn_norm_w, peer_w_query, peer_sub_keys_1, peer_sub_keys_2,
                peer_expert_down, peer_expert_up):
    f = lambda a: np.ascontiguousarray(np.asarray(a, dtype=np.float32))
    x = f(x); mem = f(mem)
    w_in0 = f(w_in)[0]
    conv_w = f(ssd_conv_w)[0]
    conv_b = f(ssd_conv_b)[0]
    w_out0 = f(w_out)[0]
    e_dn = f(peer_expert_down)[0]
    e_up = f(peer_expert_up)[0]
    wq0 = f(peer_w_query)[0]
    kk = np.zeros((128, 256), np.float32)
    kk[0:64, 0:128] = f(peer_sub_keys_1)[0].T
    kk[64:128, 128:256] = f(peer_sub_keys_2)[0].T
    perm = []
    for r in range(4):
        perm += list(range(r * 128, (r + 1) * 128))
        perm += list(range(512 + r * 64, 512 + (r + 1) * 64))
        perm += list(range(768 + r * 64, 768 + (r + 1) * 64))
    w_out_p = np.ascontiguousarray(w_out0[np.array(perm), :])
    in_maps = []
    for c in range(8):
        b, g = c // 4, c % 4
        gi = g // 2
        cols_x = np.arange(512 + g * 128, 512 + (g + 1) * 128)
        cols_B = np.arange(1024 + gi * 128, 1024 + (gi + 1) * 128)
        cols_C = np.arange(1280 + gi * 128, 1280 + (gi + 1) * 128)
        cols_z = np.arange(g * 128, (g + 1) * 128)
        cols_dt = np.arange(1536 + 2 * g, 1536 + 2 * g + 2)
        cols_q = np.arange(1544 + g * 64, 1544 + (g + 1) * 64)
        cols_k = np.arange(1800 + g * 64, 1800 + (g + 1) * 64)
        cols_v = np.arange(2056 + g * 64, 2056 + (g + 1) * 64)
        cols_xq = np.arange(2312 + g * 64, 2312 + (g + 1) * 64)
        cols = np.concatenate([cols_x, cols_B, cols_C, cols_z, cols_dt, cols_q, cols_k, cols_xq, cols_v])
        w_in_c = np.ascontiguousarray(w_in0[:, cols])
        ch = [cols_x - 512, cols_B - 512, cols_C - 512]
        convw = np.zeros((128, 12), np.float32)
        convb = np.zeros((128, 3), np.float32)
        for blk in range(3):
            convw[:, blk * 4:(blk + 1) * 4] = conv_w[:, ch[blk]].T
            convb[:, blk] = conv_b[ch[blk]]
        ssdv = np.concatenate([f(ssd_dt_bias)[0, 2 * g:2 * g + 2], f(ssd_a_log)[0, 2 * g:2 * g + 2],
                               f(ssd_d)[0, 2 * g:2 * g + 2]])[None, :]
        qkw = np.concatenate([f(moba_q_norm_w)[0], f(moba_k_norm_w)[0], f(xattn_q_norm_w)[0]])[None, :]
        wkv0 = f(w_mem_kv)[0]
        wkv = np.ascontiguousarray(np.concatenate([wkv0[:, g * 64:(g + 1) * 64], wkv0[:, 256 + g * 64:256 + (g + 1) * 64]], axis=1))
        slope = 2.0 ** (-8.0 * (g + 1) / 4)
        jj = np.arange(128, dtype=np.float64)[:, None]
        mm = np.arange(65, dtype=np.float64)[None, :]
        alibi = (slope * (jj - 128.0 * mm)).astype(np.float32)
        ridx = np.zeros((128, NT2 * 4), np.int32)
        for tt in range(NT2):
            for r in range(4):
                t0 = g * 2048 + tt * 128
                ridx[:, tt * 4 + r] = (t0 // 1024) * 4096 + r * 1024 + (t0 % 1024) + np.arange(128)
        in_maps.append({
            'x_full': x[b], 'x_res': np.ascontiguousarray(x[b, g * 2048:(g + 1) * 2048]),
            'w_in': w_in_c, 'mixw': f(mix_norm_w)[0][None, :], 'convw': convw, 'convb': convb,
            'ssdv': np.ascontiguousarray(ssdv), 'qkw': np.ascontiguousarray(qkw), 'mem': mem[b],
            'memw': f(mem_norm_w)[0][None, :], 'wkv': wkv, 'xkw': f(xattn_k_norm_w)[0][None, :],
            'alibi': alibi, 'w_out': w_out_p, 'ssdnw': f(ssd_norm_w)[0][None, :], 'ridx': ridx,
            'ffnw': f(ffn_norm_w)[0][None, :], 'wq': wq0, 'kk': kk, 'e_dn': e_dn, 'e_up': e_up,
        })
    return in_maps


def kernel(**inputs):
    in_maps = prep_inputs(**inputs)
    nc = build(debug=False)
    res = run_bass_kernel_spmd(nc, in_maps, core_ids=list(range(8)))
    outp = np.zeros((2, L, D), np.float32)
    for c in range(8):
        b, g = c // 4, c % 4
        outp[b, g * 2048:(g + 1) * 2048] = res.results[c]['out']
    return outp
```
